# Optimizing a Trainium2 kernel written in Bass

```python
import jax, jax.numpy as jnp
from jax import lax
import numpy as np

D_MODEL = 1024
BATCH = 8
SEQ = 4096
DEPTH = 1

GLA_HEADS = 4
GLA_DK = D_MODEL // 8
GLA_DV = D_MODEL // 4
GLA_RANK = 16
GLA_TAU = 16.0
GLA_CHUNK = 64
SWA_HEADS = 16
SWA_KV_HEADS = 2
SWA_HEAD_DIM = 64
SWA_WINDOW = 128
ROPE_DIM = SWA_HEAD_DIM // 4
ROPE_THETA = 500000.0
D_FF = -((-8 * D_MODEL) // (3 * 256)) * 256

GLA_QK = GLA_HEADS * GLA_DK
GLA_V = GLA_HEADS * GLA_DV
SWA_Q = SWA_HEADS * SWA_HEAD_DIM
SWA_KV = SWA_KV_HEADS * SWA_HEAD_DIM
SPLIT_SIZES = (GLA_QK, GLA_QK, GLA_V, GLA_V, GLA_RANK, SWA_Q, SWA_KV, SWA_KV, D_MODEL, D_MODEL)
D_IN = sum(SPLIT_SIZES)
SPLIT_POINTS = tuple(int(s) for s in np.cumsum(SPLIT_SIZES)[:-1])

DEEPNORM_ALPHA = (2.0 * DEPTH) ** 0.25
DEEPNORM_BETA = (8.0 * DEPTH) ** -0.25
LN_EPS = 1e-5
RMS_EPS = 1e-6

kernel_name = 'hybrid_gla_swa_sink_gated_deepnorm'


def layer_norm(x, g, b):
    xf = x.astype(jnp.float32)
    mu = jnp.mean(xf, axis=-1, keepdims=True)
    var = jnp.mean(jnp.square(xf - mu), axis=-1, keepdims=True)
    y = (xf - mu) * lax.rsqrt(var + LN_EPS) * g.astype(jnp.float32) + b.astype(jnp.float32)
    return y.astype(x.dtype)


def gla_chunked(q, k, v, log_a):
    B, T, H, dk = q.shape
    dv = v.shape[-1]
    C = GLA_CHUNK
    N = T // C

    def to_chunks(t):
        return t.astype(jnp.float32).reshape(B, N, C, H, t.shape[-1]).transpose(1, 0, 3, 2, 4)

    q, k, v, log_a = map(to_chunks, (q, k, v, log_a))
    b = jnp.cumsum(log_a, axis=-2)
    b_last = b[..., -1:, :]
    q_in = q * jnp.exp(b)
    k_in = k * jnp.exp(-b)
    k_out = k * jnp.exp(b_last - b)
    causal = jnp.tril(jnp.ones((C, C), dtype=bool))
    scores = jnp.where(causal, jnp.einsum('nbhtd,nbhsd->nbhts', q_in, k_in), 0.0)
    o_intra = jnp.einsum('nbhts,nbhsv->nbhtv', scores, v)

    def step(S, inp):
        q_c, k_c, v_c, decay = inp
        o = jnp.einsum('bhtd,bhdv->bhtv', q_c, S)
        S = S * decay[..., 0, :, None] + jnp.einsum('bhsd,bhsv->bhdv', k_c, v_c)
        return S, o

    S0 = jnp.zeros((B, H, dk, dv), jnp.float32)
    _, o_inter = lax.scan(step, S0, (q_in, k_out, v, jnp.exp(b_last)))
    o = o_intra + o_inter
    return o.transpose(1, 0, 3, 2, 4).reshape(B, T, H, dv)


def partial_rope(t, positions):
    half = ROPE_DIM // 2
    inv_freq = ROPE_THETA ** (-jnp.arange(0, ROPE_DIM, 2, dtype=jnp.float32) / ROPE_DIM)
    ang = positions.astype(jnp.float32)[..., None] * inv_freq
    cos = jnp.cos(ang)[:, :, None, :]
    sin = jnp.sin(ang)[:, :, None, :]
    tf = t.astype(jnp.float32)
    x1, x2, keep = tf[..., :half], tf[..., half:ROPE_DIM], tf[..., ROPE_DIM:]
    out = jnp.concatenate([x1 * cos - x2 * sin, x1 * sin + x2 * cos, keep], axis=-1)
    return out.astype(t.dtype)


def swa_sink_attention(q, k, v, sinks):
    B, T, Hq, Dh = q.shape
    Hkv = k.shape[2]
    G = Hq // Hkv
    W = SWA_WINDOW
    N = T // W
    qb = q.astype(jnp.float32).reshape(B, N, W, Hkv, G, Dh)
    kb = k.astype(jnp.float32).reshape(B, N, W, Hkv, Dh)
    vb = v.astype(jnp.float32).reshape(B, N, W, Hkv, Dh)

    def with_prev(t):
        prev = jnp.concatenate([jnp.zeros_like(t[:, :1]), t[:, :-1]], axis=1)
        return jnp.concatenate([prev, t], axis=2)

    k2, v2 = with_prev(kb), with_prev(vb)
    scores = jnp.einsum('bnqhgd,bnkhd->bhgnqk', qb, k2) * (Dh ** -0.5)
    q_pos = jnp.arange(W)[None, :, None] + W
    k_pos = jnp.arange(2 * W)[None, None, :]
    blk = jnp.arange(N)[:, None, None]
    valid = (k_pos <= q_pos) & (k_pos > q_pos - W) & ((blk > 0) | (k_pos >= W))
    scores = jnp.where(valid, scores, -jnp.inf)
    sink = sinks.astype(jnp.float32).reshape(Hkv, G)[None, :, :, None, None, None]
    m = jnp.maximum(jnp.max(scores, axis=-1, keepdims=True), sink)
    p = jnp.exp(scores - m)
    denom = jnp.sum(p, axis=-1, keepdims=True) + jnp.exp(sink - m)
    out = jnp.einsum('bhgnqk,bnkhd->bnqhgd', p / denom, v2)
    return out.reshape(B, T, Hq, Dh).astype(q.dtype)


def hybrid_mixer(h, positions, w_in, w_decay_up, b_decay, gla_norm_g,
                 w_branch_a, w_branch_b, sinks, w_out):
    B, T, _ = h.shape
    proj = h @ w_in
    q_a, k_a, v_a, r_a, d_low, q_b, k_b, v_b, g_a, g_b = jnp.split(proj, SPLIT_POINTS, axis=-1)

    log_a = jax.nn.log_sigmoid((d_low @ w_decay_up + b_decay).astype(jnp.float32)) / GLA_TAU
    q_a = q_a.reshape(B, T, GLA_HEADS, GLA_DK) * (GLA_DK ** -0.5)
    o_a = gla_chunked(q_a, k_a.reshape(B, T, GLA_HEADS, GLA_DK),
                      v_a.reshape(B, T, GLA_HEADS, GLA_DV),
                      log_a.reshape(B, T, GLA_HEADS, GLA_DK))
    o_a = o_a * lax.rsqrt(jnp.mean(jnp.square(o_a), axis=-1, keepdims=True) + RMS_EPS)
    o_a = o_a * gla_norm_g.astype(jnp.float32)
    o_a = (o_a.astype(h.dtype) * jax.nn.silu(r_a.reshape(B, T, GLA_HEADS, GLA_DV))).reshape(B, T, GLA_V)
    y_a = o_a @ w_branch_a

    q_b = partial_rope(q_b.reshape(B, T, SWA_HEADS, SWA_HEAD_DIM), positions)
    k_b = partial_rope(k_b.reshape(B, T, SWA_KV_HEADS, SWA_HEAD_DIM), positions)
    o_b = swa_sink_attention(q_b, k_b, v_b.reshape(B, T, SWA_KV_HEADS, SWA_HEAD_DIM), sinks)
    y_b = o_b.reshape(B, T, SWA_Q) @ w_branch_b

    merged = jax.nn.sigmoid(g_a) * y_a + jax.nn.sigmoid(g_b) * y_b
    return merged @ w_out


def swiglu_ffn(h, w_gate, w_up, w_down):
    return (jax.nn.silu(h @ w_gate) * (h @ w_up)) @ w_down


def setup_inputs(seed: int = 0) -> dict:
    key = jax.random.key(seed)
    ks = jax.random.split(key, 18)
    f32 = jnp.float32
    L = DEPTH

    def nrm(k, shape, fan_in, scale=1.0):
        return jax.random.normal(k, shape, f32) * (scale * fan_in ** -0.5)

    def gain(k, shape):
        return 1.0 + 0.02 * jax.random.normal(k, shape, f32)

    def bias(k, shape, s=0.02):
        return s * jax.random.normal(k, shape, f32)

    x = jax.random.normal(ks[0], (BATCH, SEQ, D_MODEL), f32)
    offset = jax.random.randint(ks[1], (BATCH, 1), 0, 1024, dtype=jnp.int32)
    positions = offset + jnp.arange(SEQ, dtype=jnp.int32)[None, :]
    return {
        'x': x,
        'positions': positions,
        'w_in': nrm(ks[2], (L, D_MODEL, D_IN), D_MODEL),
        'w_decay_up': nrm(ks[3], (L, GLA_RANK, GLA_QK), GLA_RANK),
        'b_decay': bias(ks[4], (L, GLA_QK), 0.1),
        'gla_norm_g': gain(ks[5], (L, GLA_DV)),
        'w_branch_a': nrm(ks[6], (L, GLA_V, D_MODEL), GLA_V),
        'w_branch_b': nrm(ks[7], (L, SWA_Q, D_MODEL), SWA_Q),
        'sinks': 0.5 * jax.random.normal(ks[8], (L, SWA_HEADS), f32),
        'w_out': nrm(ks[9], (L, D_MODEL, D_MODEL), D_MODEL, DEEPNORM_BETA),
        'ln1_g': gain(ks[10], (L, D_MODEL)),
        'ln1_b': bias(ks[11], (L, D_MODEL)),
        'w_ffn_gate': nrm(ks[12], (L, D_MODEL, D_FF), D_MODEL),
        'w_ffn_up': nrm(ks[13], (L, D_MODEL, D_FF), D_MODEL),
        'w_ffn_down': nrm(ks[14], (L, D_FF, D_MODEL), D_FF, DEEPNORM_BETA),
        'ln2_g': gain(ks[15], (L, D_MODEL)),
        'ln2_b': bias(ks[16], (L, D_MODEL)),
    }


def reference(x, positions, w_in, w_decay_up, b_decay, gla_norm_g, w_branch_a, w_branch_b,
              sinks, w_out, ln1_g, ln1_b, w_ffn_gate, w_ffn_up, w_ffn_down, ln2_g, ln2_b):
    h = x
    for layer in range(DEPTH):
        mix = hybrid_mixer(h, positions, w_in[layer], w_decay_up[layer], b_decay[layer],
                           gla_norm_g[layer], w_branch_a[layer], w_branch_b[layer],
                           sinks[layer], w_out[layer])
        h = layer_norm(DEEPNORM_ALPHA * h + mix, ln1_g[layer], ln1_b[layer])
        ffn = swiglu_ffn(h, w_ffn_gate[layer], w_ffn_up[layer], w_ffn_down[layer])
        h = layer_norm(DEEPNORM_ALPHA * h + ffn, ln2_g[layer], ln2_b[layer])
    return h
```

```python
import numpy as np
from contextlib import ExitStack
import concourse.bass as bass
import concourse.mybir as mybir
from concourse.bass_utils import run_bass_kernel_spmd

F32 = mybir.dt.float32
BF16 = mybir.dt.bfloat16
I32 = mybir.dt.int32
AF = mybir.ActivationFunctionType
ALU = mybir.AluOpType

D = 1024
TB = 512
NTL = 4
DFF = 2816
NF = DFF // 128
ALPHA = float(2.0 ** 0.25)
LN_EPS = 1e-5
RMS_EPS = 1e-6
ROPE_THETA = 500000.0
C1 = 6.28125
C2 = float(2 * np.pi - 6.28125)

ENGS = ("pe", "act", "dve", "pool", "sp")


class _Rec:
    def __getattr__(self, name):
        def f(*a, **k):
            self.call = (name, a, k)
            return None
        return f


class Prog:
    def __init__(self, nc, es):
        self.nc = nc
        self.es = es
        self.ops = {e: [] for e in ENGS}
        self.last_write = {}
        self.reads = {}
        self.waited = {e: {} for e in ENGS}
        self.dma_sems = {}
        self.eng_sem = {e: es.enter_context(nc.semaphore("sem_" + e)) for e in ENGS}
        self.n_dma_sem = 0
        self.cap = None

    def capture(self):
        self.cap = []
        return self.cap

    def end_capture(self):
        c = self.cap
        self.cap = None
        return c

    def replay(self, items):
        for it in items:
            kind, eng, call, reads, writes, semkey = it
            if kind == "op":
                self._op(eng, call, reads, writes)
            else:
                self._dma(eng, call, reads, writes, semkey)

    def _dep_wait(self, eng, tok, waits):
        if tok is None:
            return
        if tok[0] == "eng":
            _, e2, idx = tok
            if e2 == "pe" and eng == "pe":
                return
            key = ("eng", e2)
            if self.waited[eng].get(key, -1) >= idx:
                return
            self.waited[eng][key] = idx
            self.ops[e2][idx]["signal"] = True
            waits.append(("eng", e2, idx))
        else:
            _, semkey, val = tok
            key = ("dma", semkey)
            if self.waited[eng].get(key, -1) >= val:
                return
            self.waited[eng][key] = val
            waits.append(("dma", semkey, val))

    def _collect(self, eng, reads, writes):
        toks = []
        for r in reads:
            toks.append(self.last_write.get(r))
        for w in writes:
            toks.append(self.last_write.get(w))
            toks.extend(self.reads.get(w, ()))
        best = {}
        for t in toks:
            if t is None:
                continue
            k = (t[0], t[1])
            if k not in best or t[2] > best[k][2]:
                best[k] = t
        waits = []
        for t in best.values():
            self._dep_wait(eng, t, waits)
        return waits

    def _commit(self, tok, reads, writes):
        for r in reads:
            self.reads.setdefault(r, []).append(tok)
        for w in writes:
            self.last_write[w] = tok
            self.reads[w] = []

    def op(self, eng, fn, reads=(), writes=()):
        r = _Rec()
        fn(r)
        reads, writes = list(reads), list(writes)
        if self.cap is not None:
            self.cap.append(("op", eng, r.call, reads, writes, None))
        else:
            self._op(eng, r.call, reads, writes)

    def dma(self, eng, fn, reads=(), writes=(), semkey=None):
        r = _Rec()
        fn(r)
        reads, writes = list(reads), list(writes)
        if self.cap is not None:
            self.cap.append(("dma", eng, r.call, reads, writes, semkey))
        else:
            self._dma(eng, r.call, reads, writes, semkey)

    def _op(self, eng, call, reads, writes):
        waits = self._collect(eng, reads, writes)
        idx = len(self.ops[eng])
        self.ops[eng].append({"kind": "op", "call": call, "waits": waits, "signal": False})
        self._commit(("eng", eng, idx), reads, writes)

    def _dma(self, eng, call, reads, writes, semkey):
        waits = self._collect(eng, reads, writes)
        if semkey not in self.dma_sems:
            self.dma_sems[semkey] = [self.es.enter_context(self.nc.semaphore("d%d" % self.n_dma_sem)), 0]
            self.n_dma_sem += 1
        ent = self.dma_sems[semkey]
        ent[1] += 16
        self.ops[eng].append({"kind": "dma", "call": call, "waits": waits, "sem": ent[0], "signal": False})
        self._commit(("dma", semkey, ent[1]), reads, writes)

    def final_wait(self, eng, regions):
        waits = []
        for r in regions:
            self._dep_wait(eng, self.last_write.get(r), waits)
        self.ops[eng].append({"kind": "wait", "waits": waits, "signal": False})

    def emit(self):
        nc = self.nc
        val = {}
        for e in ENGS:
            c = 0
            for i, o in enumerate(self.ops[e]):
                if o["signal"]:
                    c += 1
                    val[(e, i)] = c
        handles = {"pe": "tensor", "act": "scalar", "dve": "vector", "pool": "gpsimd", "sp": "sync"}
        stats = {e: [len(self.ops[e]), 0] for e in ENGS}

        def make(e):
            def body(h):
                for o in self.ops[e]:
                    for w in o["waits"]:
                        if w[0] == "eng":
                            h.wait_ge(self.eng_sem[w[1]], val[(w[1], w[2])])
                        else:
                            h.wait_ge(self.dma_sems[w[1]][0], w[2])
                        stats[e][1] += 1
                    if o["kind"] == "wait":
                        continue
                    name, a, k = o["call"]
                    ins = getattr(h, name)(*a, **k)
                    if o["kind"] == "dma":
                        ins.then_inc(o["sem"], 16)
                    elif o["signal"]:
                        ins.then_inc(self.eng_sem[e], 1)
            return body

        with nc.Block() as block:
            for e in ENGS:
                if self.ops[e]:
                    getattr(block, handles[e])(make(e))
        return stats


def merge_skew(chains, skew):
    out = []
    n = len(chains)
    if n == 0:
        return out
    maxlen = max(len(c) for c in chains)
    for st_ in range(maxlen + skew * (n - 1)):
        for i, c in enumerate(chains):
            k = st_ - i * skew
            if 0 <= k < len(c):
                out.append(c[k])
    return out


def merge_prop(a, b):
    out = []
    ia = ib = 0
    while ia < len(a) or ib < len(b):
        if ib >= len(b) or (ia < len(a) and ia * len(b) <= ib * len(a)):
            out.append(a[ia])
            ia += 1
        else:
            out.append(b[ib])
            ib += 1
    return out


O_Q, O_K, O_V, O_R, O_QB, O_GA, O_GB, O_KB, O_VB, O_DL = 0, 512, 1024, 2048, 3072, 4096, 5120, 6144, 6272, 6400
NCST = 776


def build_program(NB):
    T = NB * TB
    nc = bass.Bass("TRN2", target_bir_lowering=False)
    dt = lambda name, shape, d, kind: nc.dram_tensor(name, shape, d, kind=kind).ap()
    x_d = dt("x", [T, D], F32, "ExternalInput")
    pos_d = dt("pos", [1, T], I32, "ExternalInput")
    win_d = dt("win", [128, 8, 6416], F32, "ExternalInput")
    wa_d = dt("wa", [128, 8, D], F32, "ExternalInput")
    wb_d = dt("wb", [128, 8, D], F32, "ExternalInput")
    wo_d = dt("wo", [128, 8, D], F32, "ExternalInput")
    wg_d = dt("wg", [128, 8, DFF], F32, "ExternalInput")
    wu_d = dt("wu", [128, 8, DFF], F32, "ExternalInput")
    wd_d = dt("wd", [128, NF, D], F32, "ExternalInput")
    wdec_d = dt("wdec", [17, 512], F32, "ExternalInput")
    gbc_d = dt("gbc", [1, D], F32, "ExternalInput")
    lnp_d = dt("lnp", [4, D], F32, "ExternalInput")
    sinks_d = dt("sinks8", [128, 8], F32, "ExternalInput")
    cst_d = dt("cst", [128, NCST], F32, "ExternalInput")
    lncol_d = dt("lncol", [128, 16], F32, "ExternalInput")
    out_d = dt("out", [T, D], F32, "ExternalOutput")
    scr_d = dt("wscr", [37, 128, 4096], BF16, "Internal")

    with ExitStack() as es:
        P = Prog(nc, es)
        sb = lambda name, shape, d: es.enter_context(nc.sbuf_tensor("s_" + name, shape, d))
        cst32 = sb("cst32", [128, NCST], F32)
        ident = sb("ident", [128, 128], BF16)
        permr = sb("permr", [128, 128], BF16)
        mle4 = sb("mle4", [128, 4, 128], BF16)
        gbc = sb("gbc", [128, D], F32)
        lng = sb("lng", [128, D], F32)
        lnb = sb("lnb", [128, D], F32)
        wdec = sb("wdec", [32, 512], F32)
        sk8 = sb("sk8", [128, 8], F32)
        lncol = sb("lncol", [128, 16], F32)
        S = sb("S", [128, 4, 256], F32)
        Sbf = [sb("Sbf%d" % i, [128, 4, 256], BF16) for i in range(2)]
        kbT = sb("kbT", [128, 8, 128], BF16)
        vbr = sb("vbr", [128, 8, 2, 128], BF16)
        onesp = sb("onesp", [128, 2, 128], BF16)
        nble4 = sb("nble4", [128, 4, 128], BF16)
        nbgt4 = sb("nbgt4", [128, 4, 128], BF16)
        dlowT = sb("dlowT", [32, 512], F32)
        dec = sb("dec", [128, 4, 4], F32)
        ss4 = sb("ss4", [128, 4, 4], F32)
        rs4 = sb("rs4", [128, 4, 4], F32)
        st4 = sb("st4", [128, 4, 2, 6], F32)
        mv4 = sb("mv4", [128, 4, 4], F32)
        junk = sb("junk", [128, 256], BF16)
        A1 = sb("A1", [128, 8 * 512], BF16)
        A2 = sb("A2", [128, 8, 512], BF16)
        A3 = sb("A3", [128, 8, 512], BF16)
        xT = sb("xT", [128, 8, 512], BF16)
        A5 = sb("A5", [128, 24 * 512], BF16)
        xbf = A1[:, :].rearrange("p (t d) -> p t d", d=D)
        oaT = A1[:, :].rearrange("p (k n) -> p k n", n=512)
        qbT = A2
        mT = A2
        obT = A3
        h1T = A3
        va = A5[:, 0:4096].rearrange("p (t d) -> p t d", d=D)
        rg = A5[:, 4096:8192].rearrange("p (t d) -> p t d", d=D)
        qinT = A5[:, 8192:10240].rearrange("p (h n) -> p h n", n=512)
        kinT = A5[:, 10240:12288].rearrange("p (h n) -> p h n", n=512)
        sga = A5[:, 0:2048].rearrange("p (j n) -> p j n", n=512)
        sgb = A5[:, 2048:4096].rearrange("p (j n) -> p j n", n=512)
        t1 = A5[:, 4096:8192].bitcast(F32).rearrange("p (j n) -> p j n", n=512)
        aT = A5[:, 0:NF * 512].rearrange("p (f n) -> p f n", n=512)
        a1k = lambda us: [("A1", u) for u in us]
        a2k = lambda us: [("A2", u) for u in us]
        a3k = lambda us: [("A3", u) for u in us]
        a5k = lambda us: [("A5", u) for u in us]
        ALL8 = list(range(8))

        sp_ = sb("sp", [128, 4, 512], F32)
        kout = sb("kout", [128, 4, 512], BF16)
        posi = sb("posi", [128, 512], I32)
        cosT = sb("cosT", [128, 512], F32)
        sinT = sb("sinT", [128, 512], F32)
        h1 = sb("h1", [128, 4, D], F32)
        oag = [sb("oag%d" % i, [128, D], BF16) for i in range(4)]
        hbf = oag
        NT32 = 5
        t32 = [sb("t32_%d" % i, [128, 512], F32) for i in range(NT32)]
        NT16 = 12
        t16 = [sb("t16_%d" % i, [128, 512], BF16) for i in range(NT16)]
        NT4K = 4
        t4k = [sb("t4k_%d" % i, [128, D], F32) for i in range(NT4K)]
        NW = 4
        wring = [sb("wr%d" % i, [128, 4096], BF16) for i in range(NW)]
        banks = [es.enter_context(nc.psum_tensor("bank%d" % i, [128, 512], F32)) for i in range(8)]

        ctr = {"b": 0, "t32": 0, "t16": 0, "t4k": 0, "oag": 0, "hbf": 0}

        def rr(name, n):
            i = ctr[name] % n
            ctr[name] += 1
            return i

        amode = {"mode": None, "vid": 0, "map": None}
        pools = {"bank": banks, "t32": t32, "t16": t16, "oag": oag}
        psize = {"bank": 8, "t32": NT32, "t16": NT16, "oag": 4}
        pctr = {"bank": "b", "t32": "t32", "t16": "t16", "oag": "oag"}

        def alloc(cls):
            if amode["mode"] is None:
                i = rr(pctr[cls], psize[cls])
                return pools[cls][i], (cls, i)
            vid = amode["vid"]
            amode["vid"] += 1
            if amode["mode"] == "virt":
                return pools[cls][0], ("v", cls, vid)
            i = amode["map"][vid]
            return pools[cls][i], (cls, i)

        def nb():
            return alloc("bank")

        def n32():
            return alloc("t32")

        def n16():
            return alloc("t16")

        def n4k():
            i = rr("t4k", NT4K)
            return t4k[i], ("t4k", i), i

        def region(build_fn):
            amode.update(mode="virt", vid=0, map=None)
            items = build_fn()
            amode["mode"] = None
            last = {}
            for idx, it in enumerate(items):
                for k in it[3] + it[4]:
                    if isinstance(k, tuple) and k[0] == "v":
                        last[k] = idx
            free = {c: list(range(psize[c])) for c in psize}
            for c in free:
                r = ctr[pctr[c]] % psize[c]
                free[c] = free[c][r:] + free[c][:r]
            mapping = {}
            for idx, it in enumerate(items):
                ks = [k for k in it[3] + it[4] if isinstance(k, tuple) and k[0] == "v"]
                for k in ks:
                    if k not in mapping:
                        if not free[k[1]]:
                            raise RuntimeError("pool %s exhausted at item %d" % (k[1], idx))
                        mapping[k] = free[k[1]].pop(0)
                for k in set(ks):
                    if last[k] == idx:
                        free[k[1]].append(mapping[k])
            vmap = {k[2]: v for k, v in mapping.items()}
            amode.update(mode="map", vid=0, map=vmap)
            items = build_fn()
            amode["mode"] = None
            P.replay(items)

        sched = []
        for b in range(NB):
            sched.append((win_d[:, :, O_KB:O_KB + 272], 8, 272))
            for g in range(2):
                sched.append((win_d[:, :, O_V + g * 512:O_V + (g + 1) * 512], 8, 512))
            for g in range(2):
                sched.append((win_d[:, :, O_R + g * 512:O_R + (g + 1) * 512], 8, 512))
            sched.append((win_d[:, :, O_Q:O_Q + 512], 8, 512))
            sched.append((win_d[:, :, O_K:O_K + 512], 8, 512))
            for g in range(2):
                sched.append((win_d[:, :, O_QB + g * 512:O_QB + (g + 1) * 512], 8, 512))
            for half in range(2):
                sched.append((win_d[:, :, O_GA + half * 512:O_GA + (half + 1) * 512], 8, 512))
                sched.append((win_d[:, :, O_GB + half * 512:O_GB + (half + 1) * 512], 8, 512))
                sched.append((wa_d[:, :, half * 512:(half + 1) * 512], 8, 512))
                sched.append((wb_d[:, :, half * 512:(half + 1) * 512], 8, 512))
            for half in range(2):
                sched.append((wo_d[:, :, half * 512:(half + 1) * 512], 8, 512))
            for i in range(6):
                nf = 4 if i < 5 else 2
                sched.append((wg_d[:, :, i * 512:i * 512 + nf * 128], 8, nf * 128))
                sched.append((wu_d[:, :, i * 512:i * 512 + nf * 128], 8, nf * 128))
            for i in range(6):
                nf = 4 if i < 5 else 2
                sched.append((wd_d[:, 4 * i:4 * i + nf, :], nf, D))
        NGB = len(sched) // NB
        assert NGB == 37
        wst = {"issued": 0, "next": 0}

        def wview(j):
            _, k, n = sched[j]
            return wring[j % NW][:, 0:k * n].rearrange("p (k n) -> p k n", n=n)

        def wget(live=1):
            assert P.cap is None
            j = wst["next"]
            wst["next"] += 1
            while wst["issued"] < min(len(sched), j + NW - live + 1):
                i = wst["issued"]
                src, k, n = sched[i]
                dst = wview(i)
                flat = wring[i % NW][:, 0:k * n]
                g = i % NGB
                if i < NGB:
                    P.dma("pool", lambda h: h.dma_start(out=dst, in_=src), writes=[("w", i % NW)], semkey=("w", i % NW))
                    if NB > 1:
                        P.dma("sp", lambda h: h.dma_start(out=scr_d[g, :, 0:k * n], in_=flat), reads=[("w", i % NW)],
                              writes=[("scr", g)], semkey=("scr", g))
                else:
                    P.dma("pool", lambda h: h.dma_start(out=flat, in_=scr_d[g, :, 0:k * n]), reads=[("scr", g)],
                          writes=[("w", i % NW)], semkey=("w", i % NW))
                wst["issued"] += 1
            return wview(j), ("w", j % NW)

        P.dma("sp", lambda h: h.dma_start(out=cst32[:, :], in_=cst_d), writes=["cst"], semkey="cst")
        P.dma("sp", lambda h: h.dma_start(out=gbc[:, :], in_=gbc_d.partition_broadcast(128)), writes=["gbc"], semkey="gbc")
        P.dma("sp", lambda h: h.dma_start(out=wdec[0:17, :], in_=wdec_d), writes=["wdec"], semkey="wdec")
        P.dma("sp", lambda h: h.dma_start(out=sk8[:, :], in_=sinks_d), writes=["sk8"], semkey="sk8")
        P.dma("sp", lambda h: h.dma_start(out=lncol[:, :], in_=lncol_d), writes=["lncol"], semkey="lncol")
        P.op("dve", lambda h: h.tensor_copy(out=ident[:, :], in_=cst32[:, 0:128]), reads=["cst"], writes=["ident"])
        P.op("dve", lambda h: h.tensor_copy(out=permr[:, :], in_=cst32[:, 128:256]), reads=["cst"], writes=["permr"])
        for j in range(4):
            P.op("dve", lambda h, j=j: h.tensor_copy(out=mle4[:, j, :], in_=cst32[:, 256:384]), reads=["cst"], writes=["mle4"])
        P.op("act", lambda h: h.activation(out=sk8[:, :], in_=sk8[:, :], func=AF.Exp), reads=["sk8"], writes=["sk8"])
        for j in range(4):
            P.op("dve", lambda h, j=j: h.tensor_scalar(out=nble4[:, j, :], in0=cst32[:, 256:384], scalar1=-1.0, scalar2=30000.0, op0=ALU.add, op1=ALU.mult),
                 reads=["cst"], writes=["nble4"])
            P.op("dve", lambda h, j=j: h.tensor_scalar(out=nbgt4[:, j, :], in0=cst32[:, 384:512], scalar1=-1.0, scalar2=30000.0, op0=ALU.add, op1=ALU.mult),
                 reads=["cst"], writes=["nbgt4"])
        P.op("dve", lambda h: h.memset(dlowT[:, :], 1.0), writes=["dlowT"])
        P.op("dve", lambda h: h.memset(S[:, :, :], 0.0), writes=[("S", hh) for hh in range(4)])
        for i2 in range(2):
            P.op("dve", lambda h, i2=i2: h.memset(Sbf[i2][:, :, :], 0.0), writes=[("Sbf", i2, hh) for hh in range(4)])
        P.op("dve", lambda h: h.memset(onesp[:, :, :], 0.0), writes=["onesp"])
        P.op("dve", lambda h: h.memset(onesp[:, 0, 0:64], 1.0), reads=["onesp"], writes=["onesp"])
        P.op("dve", lambda h: h.memset(onesp[:, 1, 64:128], 1.0), reads=["onesp"], writes=["onesp"])
        P.op("dve", lambda h: h.memset(vbr[:, :, :, :], 0.0), writes=[("vbr", i) for i in range(8)])
        invf = cst32[:, 768:769]
        tri_inc = cst32[:, 512:640]
        tri_rev = cst32[:, 640:768]

        def load_x(b):
            src = x_d[b * TB:(b + 1) * TB, :].rearrange("(t p) d -> p t d", p=128)
            P.dma("pool", lambda h: h.dma_start(out=xbf, in_=src), writes=a1k(ALL8), semkey="xbf")

        load_x(0)

        def ln_stats(xs, xkey, t):
            stt, mvt = st4[:, t, :, :], mv4[:, t, :]
            sk_, mk_ = ("st", t), ("mv", t)
            P.op("dve", lambda h: h.bn_stats(out=stt[:, 0, :], in_=xs[:, 0:512]), reads=[xkey], writes=[sk_])
            P.op("dve", lambda h: h.bn_stats(out=stt[:, 1, :], in_=xs[:, 512:1024]), reads=[xkey], writes=[sk_])
            P.op("dve", lambda h: h.bn_aggr(out=mvt[:, 0:2], in_=stt), reads=[sk_], writes=[mk_])
            P.op("dve", lambda h: h.tensor_scalar(out=mvt[:, 2:3], in0=mvt[:, 1:2], scalar1=LN_EPS, scalar2=None, op0=ALU.add),
                 reads=[mk_], writes=[mk_])
            P.op("act", lambda h: h.activation(out=mvt[:, 2:3], in_=mvt[:, 2:3], func=AF.Sqrt), reads=[mk_], writes=[mk_])
            P.op("dve", lambda h: h.reciprocal(out=mvt[:, 2:3], in_=mvt[:, 2:3]), reads=[mk_], writes=[mk_])
            return mvt, mk_

        def ln_affine(xs, xkey, dst, dkey, mvt, mk_):
            P.op("dve", lambda h: h.tensor_scalar(out=xs[:, :], in0=xs[:, :], scalar1=mvt[:, 0:1], scalar2=mvt[:, 2:3],
                                                  op0=ALU.subtract, op1=ALU.mult), reads=[xkey, mk_], writes=[xkey])
            P.op("pool", lambda h: h.tensor_tensor(out=xs[:, :], in0=xs[:, :], in1=lng[:, :], op=ALU.mult),
                 reads=[xkey, "lng"], writes=[xkey])
            P.op("pool", lambda h: h.tensor_tensor(out=dst, in0=xs[:, :], in1=lnb[:, :], op=ALU.add),
                 reads=[xkey, "lnb"], writes=[dkey] if dkey != xkey else [xkey])

        def load_ln(row):
            P.dma("sp", lambda h: h.dma_start(out=lng[:, :], in_=lnp_d[row:row + 1, :].partition_broadcast(128)),
                  writes=["lng"], semkey="lng")
            P.dma("sp", lambda h: h.dma_start(out=lnb[:, :], in_=lnp_d[row + 1:row + 2, :].partition_broadcast(128)),
                  writes=["lnb"], semkey="lnb")

        def mm_acc(bank, bkey, lhs_fn, rhs_fn, nk, reads):
            for kc in range(nk):
                P.op("pe", lambda h, kc=kc: h.matmul(bank, lhsT=lhs_fn(kc), rhs=rhs_fn(kc), start=(kc == 0), stop=(kc == nk - 1)),
                     reads=reads, writes=[bkey])

        def flat4(m):
            return m[:, :, :].rearrange("p a c -> p (a c)")

        out_keys = []
        pending = {"ln2": []}
        XT_ALL = [("xT", k) for k in range(8)]

        for b in range(NB):
            P.dma("sp", lambda h, b=b: h.dma_start(out=posi[:, :], in_=pos_d[0:1, b * TB:(b + 1) * TB].partition_broadcast(128)),
                  writes=["posi"], semkey="posi")
            for kcp in range(4):
                bank, bkey = nb()
                bv = bank[:, :].bitcast(BF16)
                for j in range(2):
                    kc = 2 * kcp + j
                    for t in range(4):
                        P.op("pe", lambda h, j=j, t=t, kc=kc, bv=bv: h.transpose(bv[:, j * 512 + t * 128:j * 512 + (t + 1) * 128],
                                                                               xbf[:, t, kc * 128:(kc + 1) * 128], ident[:, :]),
                             reads=a1k([2 * t, 2 * t + 1]) + ["ident"], writes=[bkey])
                P.op("act", lambda h, kcp=kcp, bv=bv: h.activation(out=xT[:, 2 * kcp:2 * kcp + 2, :].rearrange("p k n -> p (k n)"),
                                                                  in_=bv, func=AF.Copy),
                     reads=[bkey], writes=[("xT", 2 * kcp), ("xT", 2 * kcp + 1)])
            ta, tak = n32()
            tki, tck = posi[:, :], "posi"
            P.op("dve", lambda h: h.tensor_copy(out=ta[:, :], in_=posi[:, :]), reads=["posi"], writes=[tak])
            P.op("dve", lambda h: h.tensor_scalar(out=ta[:, :], in0=ta[:, :], scalar1=invf, scalar2=None, op0=ALU.mult),
                 reads=[tak, "cst"], writes=[tak])
            P.op("dve", lambda h: h.tensor_scalar(out=tki, in0=ta[:, :], scalar1=float(1.0 / (2 * np.pi)), scalar2=None, op0=ALU.mult),
                 reads=[tak], writes=[tck])
            tb_, tbk = n32()
            P.op("dve", lambda h: h.tensor_copy(out=tb_[:, :], in_=tki), reads=[tck], writes=[tbk])
            P.op("dve", lambda h: h.scalar_tensor_tensor(out=ta[:, :], in0=tb_[:, :], scalar=-C1, in1=ta[:, :], op0=ALU.mult, op1=ALU.add),
                 reads=[tak, tbk], writes=[tak])
            P.op("dve", lambda h: h.scalar_tensor_tensor(out=ta[:, :], in0=tb_[:, :], scalar=-C2, in1=ta[:, :], op0=ALU.mult, op1=ALU.add),
                 reads=[tak, tbk], writes=[tak])
            P.op("dve", lambda h: h.tensor_scalar(out=tb_[:, :], in0=ta[:, :], scalar1=float(np.pi), scalar2=float(-2 * np.pi),
                                                  op0=ALU.is_gt, op1=ALU.mult), reads=[tak], writes=[tbk])
            P.op("dve", lambda h: h.tensor_tensor(out=ta[:, :], in0=ta[:, :], in1=tb_[:, :], op=ALU.add), reads=[tak, tbk], writes=[tak])
            P.op("act", lambda h: h.activation(out=sinT[:, :], in_=ta[:, :], func=AF.Sin), reads=[tak], writes=["sinT"])
            P.op("dve", lambda h: h.scalar_tensor_tensor(out=tb_[:, :], in0=ta[:, :], scalar=-1.0, in1=ta[:, :], op0=ALU.mult, op1=ALU.max), reads=[tak], writes=[tbk])
            P.op("act", lambda h: h.activation(out=cosT[:, :], in_=tb_[:, :], func=AF.Sin, scale=-1.0, bias=float(np.pi / 2)),
                 reads=[tbk], writes=["cosT"])

            def rope_head(bank, bkey):
                raw, rk = n16()
                P.op("act", lambda h: h.activation(out=raw[:, :], in_=bank[:, :], func=AF.Copy), reads=[bkey], writes=[rk])
                return raw, rk

            def rope_tail(raw, rk, dst, dkeys):
                b2, b2k = nb()
                P.op("pe", lambda h: h.matmul(b2[:, :], lhsT=permr[:, :], rhs=raw[:, :], start=True, stop=True),
                     reads=["permr", rk], writes=[b2k])
                ts, tsk = n32()
                P.op("dve", lambda h: h.tensor_tensor(out=ts[:, :], in0=b2[:, :], in1=sinT[:, :], op=ALU.mult),
                     reads=[b2k, "sinT"], writes=[tsk])
                tcs, tcsk = n32()
                P.op("pool", lambda h: h.tensor_tensor(out=tcs[:, :], in0=raw[:, :], in1=cosT[:, :], op=ALU.mult),
                     reads=[rk, "cosT"], writes=[tcsk])
                P.op("pool", lambda h: h.tensor_tensor(out=dst, in0=tcs[:, :], in1=ts[:, :], op=ALU.add),
                     reads=[tcsk, tsk], writes=dkeys)

            wm, wmk = wget()
            s0 = (4 * b) % 8
            bankKB, bKBk = nb()
            mm_acc(bankKB[:, :], bKBk, lambda kc: wm[:, kc, 0:128], lambda kc: xT[:, kc, :], 8, [wmk] + XT_ALL)
            kraw, krk = rope_head(bankKB, bKBk)
            bank, bkey = nb()
            for t in range(4):
                mm_acc(bank[:, t * 128:(t + 1) * 128], bkey, lambda kc, t=t: xT[:, kc, t * 128:(t + 1) * 128],
                       lambda kc: wm[:, kc, 128:256], 8, [wmk] + XT_ALL)
            bk3 = bank[:, :].rearrange("p (t c) -> p t c", c=128)
            for g in range(2):
                P.op("act", lambda h, g=g: h.activation(out=vbr[:, s0:s0 + 4, g, 64 * g:64 * g + 64], in_=bk3[:, :, 64 * g:64 * g + 64], func=AF.Copy),
                     reads=[bkey] + [("vbr", s0 + i) for i in range(4)], writes=[("vbr", s0 + i) for i in range(4)])
            bank, bkey = nb()
            mm_acc(bank[0:16, :], bkey, lambda kc: wm[:, kc, 256:272], lambda kc: xT[:, kc, :], 8, [wmk] + XT_ALL)
            P.op("act", lambda h, bank=bank: h.activation(out=dlowT[0:16, :], in_=bank[0:16, :], func=AF.Copy), reads=[bkey], writes=["dlowT"])

            ln2_items = pending["ln2"]
            pending["ln2"] = []
            npiece = 16
            pieces = [ln2_items[(len(ln2_items) * i) // npiece:(len(ln2_items) * (i + 1)) // npiece] for i in range(npiece)]
            for g in range(2):
                wv, wvk = wget()
                for t in range(4):
                    bank, bkey = nb()
                    mm_acc(bank[:, :], bkey, lambda kc, t=t: xT[:, kc, t * 128:(t + 1) * 128], lambda kc: wv[:, kc, :], 8, [wvk] + XT_ALL)
                    P.op("act", lambda h, bank=bank, t=t, g=g: h.activation(out=va[:, t, g * 512:(g + 1) * 512], in_=bank[:, :], func=AF.Copy),
                         reads=[bkey], writes=a5k([2 * t + g]))
                    P.replay(pieces.pop(0))
                if g == 0:
                    rope_tail(kraw, krk, kbT[:, s0:s0 + 4, :].rearrange("p s n -> p (s n)"), [("kbT", s0 + i) for i in range(4)])
                    for t in range(4):
                        bank, bkey = nb()
                        P.op("pe", lambda h, bank=bank, t=t: h.matmul(bank[:, :], lhsT=dlowT[0:17, t * 128:(t + 1) * 128], rhs=wdec[0:17, :],
                                                                      start=True, stop=True), reads=["dlowT", "wdec"], writes=[bkey])
                        ez, ezk = n32()
                        P.op("act", lambda h, bank=bank, ez=ez: h.activation(out=ez[:, :], in_=bank[:, :], func=AF.Exp, scale=-1.0),
                             reads=[bkey], writes=[ezk])
                        P.op("act", lambda h, ez=ez, t=t: h.activation(out=sp_[:, t, :], in_=ez[:, :], func=AF.Ln, bias=1.0),
                             reads=[ezk], writes=[("sp", t)])
            for g in range(2):
                wr_, wrk = wget()
                for t in range(4):
                    bank, bkey = nb()
                    mm_acc(bank[:, :], bkey, lambda kc, t=t: xT[:, kc, t * 128:(t + 1) * 128], lambda kc: wr_[:, kc, :], 8, [wrk] + XT_ALL)
                    sr, srk = n32()
                    P.op("act", lambda h, bank=bank, sr=sr: h.activation(out=sr[:, :], in_=bank[:, :], func=AF.Silu), reads=[bkey], writes=[srk])
                    P.op("pool", lambda h, sr=sr, t=t, g=g: h.tensor_tensor(out=rg[:, t, g * 512:(g + 1) * 512], in0=sr[:, :],
                                                                           in1=gbc[:, g * 512:(g + 1) * 512], op=ALU.mult),
                         reads=[srk, "gbc"], writes=a5k([8 + 2 * t + g]))
                    P.replay(pieces.pop(0))

            wq, wqk = wget()
            wk, wkk = wget(live=2)
            for hd in range(4):
                bank, bkey = nb()
                for t in range(4):
                    P.op("pe", lambda h, bank=bank, t=t, hd=hd: h.matmul(bank[:, t * 128:(t + 1) * 128], lhsT=sp_[:, t, hd * 128:(hd + 1) * 128],
                                                                         rhs=tri_inc, start=True, stop=True),
                         reads=[("sp", t), "cst"], writes=[bkey])
                eq, eqk = n32()
                ek, ekk = n32()
                P.op("act", lambda h, bank=bank, eq=eq: h.activation(out=eq[:, :], in_=bank[:, :], func=AF.Exp), reads=[bkey], writes=[eqk])
                P.op("act", lambda h, bank=bank, ek=ek: h.activation(out=ek[:, :], in_=bank[:, :], func=AF.Exp, scale=-1.0), reads=[bkey], writes=[ekk])
                P.op("dve", lambda h, eq=eq, hd=hd: h.tensor_copy(out=dec[:, hd, :], in_=eq[:, :].rearrange("p (t c) -> p t c", c=128)[:, :, 127]),
                     reads=[eqk], writes=[("dec", hd)])
                bank, bkey = nb()
                mm_acc(bank[:, :], bkey, lambda kc, hd=hd: wq[:, kc, hd * 128:(hd + 1) * 128], lambda kc: xT[:, kc, :], 8, [wqk] + XT_ALL)
                P.op("dve", lambda h, bank=bank, eq=eq, hd=hd: h.scalar_tensor_tensor(out=qinT[:, hd, :], in0=bank[:, :], scalar=float(128 ** -0.5),
                                                                                      in1=eq[:, :], op0=ALU.mult, op1=ALU.mult),
                     reads=[bkey, eqk], writes=a5k([16 + hd]))
                bank, bkey = nb()
                mm_acc(bank[:, :], bkey, lambda kc, hd=hd: wk[:, kc, hd * 128:(hd + 1) * 128], lambda kc: xT[:, kc, :], 8, [wkk] + XT_ALL)
                P.op("dve", lambda h, bank=bank, ek=ek, hd=hd: h.tensor_tensor(out=kinT[:, hd, :], in0=bank[:, :], in1=ek[:, :], op=ALU.mult),
                     reads=[bkey, ekk], writes=a5k([20 + hd]))
            for t in range(4):
                bank, bkey = nb()
                P.op("pe", lambda h, bank=bank, t=t: h.matmul(bank[:, :], lhsT=tri_rev, rhs=sp_[:, t, :], start=True, stop=True),
                     reads=[("sp", t), "cst"], writes=[bkey])
                er, erk = n32()
                P.op("act", lambda h, bank=bank, er=er: h.activation(out=er[:, :], in_=bank[:, :], func=AF.Exp), reads=[bkey], writes=[erk])
                bank, bkey = nb()
                mm_acc(bank[:, :], bkey, lambda kc, t=t: xT[:, kc, t * 128:(t + 1) * 128], lambda kc: wk[:, kc, :], 8, [wkk] + XT_ALL)
                P.op("dve", lambda h, bank=bank, er=er, t=t: h.tensor_tensor(out=kout[:, t, :], in0=bank[:, :], in1=er[:, :], op=ALU.mult),
                     reads=[bkey, erk], writes=[("kout", t)])

            for g in range(2):
                wqb, wqbk = wget()

                def build_qb(g=g, wqb=wqb, wqbk=wqbk):
                    chains = []
                    for j in range(4):
                        p = 4 * g + j
                        P.capture()
                        bank, bkey = nb()
                        mm_acc(bank[:, :], bkey, lambda kc, j=j: wqb[:, kc, j * 128:(j + 1) * 128], lambda kc: xT[:, kc, :], 8, [wqbk] + XT_ALL)
                        raw, rk = rope_head(bank, bkey)
                        rope_tail(raw, rk, qbT[:, p, :], a2k([p]))
                        chains.append(P.end_capture())
                    return merge_skew(chains, 9)
                region(build_qb)

            def build_mix(b=b):
                g_heads, g_tailA, g_tailB = [], [], []
                for t in range(4):
                    n = 4 * b + t
                    tsl = slice(t * 128, (t + 1) * 128)
                    sb_prev, sb_cur = Sbf[(n + 1) % 2], Sbf[n % 2]
                    kp, kc_ = (n + 1) % 2, n % 2
                    P.capture()
                    bankS, bSk = nb()
                    for hd in range(4):
                        P.op("pe", lambda h, hd=hd: h.matmul(bankS[:, hd * 128:(hd + 1) * 128], lhsT=kinT[:, hd, tsl],
                                                             rhs=qinT[:, hd, tsl], start=True, stop=True),
                             reads=a5k([16 + hd, 20 + hd]), writes=[bSk])
                    sc, sck = n16()
                    P.op("dve", lambda h: h.tensor_tensor(out=sc[:, :], in0=bankS[:, :], in1=flat4(mle4), op=ALU.mult),
                         reads=[bSk, "mle4"], writes=[sck])
                    for hp in range(2):
                        bankD, bDk = nb()
                        for hh in range(2):
                            hd = 2 * hp + hh
                            P.op("pe", lambda h, hd=hd, hh=hh: h.matmul(bankD[:, hh * 256:(hh + 1) * 256], lhsT=kout[:, t, hd * 128:(hd + 1) * 128],
                                                                        rhs=va[:, t, hd * 256:(hd + 1) * 256], start=True, stop=True),
                                 reads=[("kout", t)] + a5k([2 * t + hd // 2]), writes=[bDk])
                        for hh in range(2):
                            hd = 2 * hp + hh
                            P.op("dve", lambda h, hd=hd, hh=hh: h.scalar_tensor_tensor(out=S[:, hd, :], in0=S[:, hd, :], scalar=dec[:, hd, t:t + 1],
                                                                                       in1=bankD[:, hh * 256:(hh + 1) * 256], op0=ALU.mult, op1=ALU.add),
                                 reads=[("S", hd), ("dec", hd), bDk], writes=[("S", hd)])
                            P.op("act", lambda h, hd=hd: h.activation(out=sb_cur[:, hd, :], in_=S[:, hd, :], func=AF.Copy),
                                 reads=[("S", hd)], writes=[("Sbf", kc_, hd)])
                    bo = []
                    for hp in range(2):
                        bankO, bOk = nb()
                        bo.append((bankO, bOk))
                        for hh in range(2):
                            hd = 2 * hp + hh
                            P.op("pe", lambda h, hd=hd, hh=hh: h.matmul(bankO[:, hh * 256:(hh + 1) * 256], lhsT=sc[:, hd * 128:(hd + 1) * 128],
                                                                        rhs=va[:, t, hd * 256:(hd + 1) * 256], start=True, stop=False),
                                 reads=[sck] + a5k([2 * t + hd // 2]), writes=[bOk])
                            P.op("pe", lambda h, hd=hd, hh=hh: h.matmul(bankO[:, hh * 256:(hh + 1) * 256], lhsT=qinT[:, hd, tsl],
                                                                        rhs=sb_prev[:, hd, :], start=False, stop=True),
                                 reads=a5k([16 + hd]) + [("Sbf", kp, hd)], writes=[bOk])
                    g_heads.append(P.end_capture())
                    P.capture()
                    sst, rst = ss4[:, t, :], rs4[:, t, :]
                    for hd in range(4):
                        bankO, bOk = bo[hd // 2]
                        hh = hd % 2
                        P.op("act", lambda h, hd=hd, hh=hh: h.activation(out=junk[:, :], in_=bankO[:, hh * 256:(hh + 1) * 256], func=AF.Square,
                                                                         accum_out=sst[:, hd:hd + 1]), reads=[bOk], writes=[("ss", t)])
                    P.op("dve", lambda h: h.tensor_scalar(out=rst, in0=sst, scalar1=float(1.0 / 256), scalar2=RMS_EPS, op0=ALU.mult, op1=ALU.add),
                         reads=[("ss", t)], writes=[("rs", t)])
                    P.op("act", lambda h: h.activation(out=rst, in_=rst, func=AF.Sqrt), reads=[("rs", t)], writes=[("rs", t)])
                    P.op("dve", lambda h: h.reciprocal(out=rst, in_=rst), reads=[("rs", t)], writes=[("rs", t)])
                    og, ogk = alloc("oag")
                    for hd in range(4):
                        bankO, bOk = bo[hd // 2]
                        hh = hd % 2
                        P.op("dve", lambda h, hd=hd, hh=hh: h.scalar_tensor_tensor(out=og[:, hd * 256:(hd + 1) * 256], in0=bankO[:, hh * 256:(hh + 1) * 256],
                                                                                   scalar=rst[:, hd:hd + 1], in1=rg[:, t, hd * 256:(hd + 1) * 256],
                                                                                   op0=ALU.mult, op1=ALU.mult),
                             reads=[bOk, ("rs", t)] + a5k([8 + 2 * t + hd // 2]), writes=[ogk])
                    g_tailA.append(P.end_capture())
                    P.capture()
                    bankT, bTk = nb()
                    btv = bankT[:, :].bitcast(BF16)
                    for c in range(8):
                        P.op("pe", lambda h, c=c: h.transpose(btv[:, c * 128:(c + 1) * 128], og[:, c * 128:(c + 1) * 128], ident[:, :]),
                             reads=[ogk, "ident"], writes=[bTk])
                    P.op("dve", lambda h: h.tensor_copy(out=oaT[:, :, tsl], in_=btv.rearrange("p (k n) -> p k n", n=128)),
                         reads=[bTk], writes=a1k(ALL8))
                    g_tailB.append(P.end_capture())

                w_heads, w_tails = [], []
                for t in range(4):
                    n = 4 * b + t
                    tsl = slice(t * 128, (t + 1) * 128)
                    sl_cur = n % 8
                    sl_prev = (n - 1) % 8
                    js = ([("prev", sl_prev)] if n > 0 else []) + [("cur", sl_cur)]
                    for hh in range(2):
                        pts = {}
                        P.capture()
                        for (nm, slj) in js:
                            for g in range(2):
                                gs = slice(64 * g, 64 * g + 64)
                                bank, bkey = nb()
                                P.op("pe", lambda h: h.matmul(bank[:, :].rearrange("p (a c) -> p a c", c=128),
                                                              lhsT=kbT[gs, slj, :], rhs=qbT[gs, 4 * hh:4 * hh + 4, tsl],
                                                              start=True, stop=False),
                                     reads=[("kbT", slj)] + a2k(range(4 * hh, 4 * hh + 4)), writes=[bkey])
                                msk = nbgt4 if nm == "prev" else nble4
                                mk = "nbgt4" if nm == "prev" else "nble4"
                                P.op("pe", lambda h: h.matmul(bank[:, :], lhsT=ident[:, :], rhs=flat4(msk), start=False, stop=True),
                                     reads=["ident", mk], writes=[bkey])
                                pt, ptk = n16()
                                P.op("act", lambda h: h.activation(out=pt[:, :], in_=bank[:, :], func=AF.Exp, scale=0.125),
                                     reads=[bkey], writes=[ptk])
                                pts[(g, nm)] = (pt, ptk, slj)
                        w_heads.append(P.end_capture())
                        P.capture()
                        bankN, bNk = nb()
                        bankDn, bDnk = nb()
                        combos = [(g, nm, slj) for g in range(2) for (nm, slj) in js]
                        for idx, (g, nm, slj) in enumerate(combos):
                            pt, ptk, _ = pts[(g, nm)]
                            first = idx == 0
                            last = idx == len(combos) - 1
                            P.op("pe", lambda h: h.matmul(bankN[:, :], lhsT=vbr[:, slj, g, :], rhs=pt[:, :], start=first, stop=last),
                                 reads=[("vbr", slj), ptk], writes=[bNk])
                            P.op("pe", lambda h: h.matmul(bankDn[:, :], lhsT=onesp[:, g, :], rhs=pt[:, :], start=first, stop=last),
                                 reads=["onesp", ptk], writes=[bDnk])
                        den, denk = n32()
                        P.op("dve", lambda h: h.tensor_tensor(out=den[:, :].rearrange("p (a c) -> p a c", c=128),
                                                              in0=bankDn[:, :].rearrange("p (a c) -> p a c", c=128),
                                                              in1=sk8[:, 4 * hh:4 * hh + 4].unsqueeze(2).to_broadcast([128, 4, 128]), op=ALU.add),
                             reads=[bDnk, "sk8"], writes=[denk])
                        P.op("act", lambda h: h.activation(out=den[:, :], in_=den[:, :], func=AF.Ln), reads=[denk], writes=[denk])
                        P.op("act", lambda h: h.activation(out=den[:, :], in_=den[:, :], func=AF.Exp, scale=-1.0), reads=[denk], writes=[denk])
                        P.op("dve", lambda h: h.tensor_tensor(out=obT[:, 4 * hh:4 * hh + 4, tsl],
                                                              in0=bankN[:, :].rearrange("p (a c) -> p a c", c=128),
                                                              in1=den[:, :].rearrange("p (a c) -> p a c", c=128), op=ALU.mult),
                             reads=[bNk, denk], writes=a3k(range(4 * hh, 4 * hh + 4)))
                        w_tails.append(P.end_capture())
                seqA = []
                for t in range(4):
                    seqA += g_heads[t] + g_tailA[t]
                    if t > 0:
                        seqA += g_tailB[t - 1]
                seqA += g_tailB[3]
                seqB = []
                for u in range(8):
                    seqB += w_heads[u]
                    if u > 0:
                        seqB += w_tails[u - 1]
                seqB += w_tails[7]
                return merge_prop(seqA, seqB)
            region(build_mix)

            load_ln(0)
            for half in range(2):
                wga, wgak = wget()
                for j in range(4):
                    bank, bkey = nb()
                    mm_acc(bank[:, :], bkey, lambda kc, j=j: wga[:, kc, j * 128:(j + 1) * 128], lambda kc: xT[:, kc, :], 8, [wgak] + XT_ALL)
                    P.op("act", lambda h, bank=bank, j=j: h.activation(out=sga[:, j, :], in_=bank[:, :], func=AF.Sigmoid), reads=[bkey], writes=a5k([j]))
                wgb, wgbk = wget()
                for j in range(4):
                    bank, bkey = nb()
                    mm_acc(bank[:, :], bkey, lambda kc, j=j: wgb[:, kc, j * 128:(j + 1) * 128], lambda kc: xT[:, kc, :], 8, [wgbk] + XT_ALL)
                    P.op("act", lambda h, bank=bank, j=j: h.activation(out=sgb[:, j, :], in_=bank[:, :], func=AF.Sigmoid), reads=[bkey], writes=a5k([4 + j]))
                wa_, wak = wget()
                for j in range(4):
                    bank, bkey = nb()
                    mm_acc(bank[:, :], bkey, lambda kc, j=j: wa_[:, kc, j * 128:(j + 1) * 128], lambda kc: oaT[:, kc, :], 8, [wak] + a1k(ALL8))
                    P.op("dve", lambda h, bank=bank, j=j: h.tensor_tensor(out=t1[:, j, :], in0=bank[:, :], in1=sga[:, j, :], op=ALU.mult),
                         reads=[bkey] + a5k([j]), writes=a5k([8 + 2 * j, 9 + 2 * j]))
                wb_, wbk = wget()
                for j in range(4):
                    c = 4 * half + j
                    bank, bkey = nb()
                    mm_acc(bank[:, :], bkey, lambda kc, j=j: wb_[:, kc, j * 128:(j + 1) * 128], lambda kc: obT[:, kc, :], 8, [wbk] + a3k(ALL8))
                    t2, t2k = n32()
                    P.op("dve", lambda h, bank=bank, j=j, t2=t2: h.tensor_tensor(out=t2[:, :], in0=bank[:, :], in1=sgb[:, j, :], op=ALU.mult),
                         reads=[bkey] + a5k([4 + j]), writes=[t2k])
                    P.op("pool", lambda h, j=j, c=c, t2=t2: h.tensor_tensor(out=mT[:, c, :], in0=t1[:, j, :], in1=t2[:, :], op=ALU.add),
                         reads=[t2k] + a5k([8 + 2 * j, 9 + 2 * j]), writes=a2k([c]))
            if b + 1 < NB:
                load_x(b + 1)

            wo0, wo0k = wget()
            wo1, wo1k = wget(live=2)
            def build_ln1(b=b):
                bq = [nb() for _ in range(4)]
                A, H, T, F = [], [], [], []
                for t in range(4):
                    tsl = slice(t * 128, (t + 1) * 128)
                    xs, xsk, xsi = n4k()
                    P.capture()
                    P.dma("sp", lambda h: h.dma_start(out=xs[:, :], in_=x_d[b * TB + t * 128:b * TB + (t + 1) * 128, :]),
                          writes=[xsk], semkey=("t4k_in", xsi))
                    for half, (wo_, wok) in enumerate(((wo0, wo0k), (wo1, wo1k))):
                        bank, bkey = nb()
                        mm_acc(bank[:, :], bkey, lambda kc: mT[:, kc, tsl], lambda kc: wo_[:, kc, :], 8, [wok] + a2k(ALL8))
                        P.op("dve", lambda h: h.scalar_tensor_tensor(out=xs[:, half * 512:(half + 1) * 512], in0=xs[:, half * 512:(half + 1) * 512],
                                                                     scalar=ALPHA, in1=bank[:, :], op0=ALU.mult, op1=ALU.add),
                             reads=[bkey, xsk], writes=[xsk])
                    A.append(P.end_capture())
                    P.capture()
                    mvt, mk_ = ln_stats(xs, xsk, t)
                    P.op("dve", lambda h: h.scalar_tensor_tensor(out=mvt[:, 3:4], in0=mvt[:, 0:1], scalar=-1.0, in1=mvt[:, 2:3],
                                                                 op0=ALU.mult, op1=ALU.mult), reads=[mk_], writes=[mk_])
                    hb, hbk = alloc("oag")
                    P.op("act", lambda h: h.activation(out=hb[:, :], in_=xs[:, :], func=AF.Identity, scale=mvt[:, 2:3], bias=mvt[:, 3:4]),
                         reads=[xsk, mk_], writes=[hbk])
                    H.append(P.end_capture())
                    P.capture()
                    for c in range(8):
                        bank, bkey = bq[c // 2]
                        btv = bank[:, :].bitcast(BF16)
                        o0 = (c % 2) * 512 + t * 128
                        P.op("pe", lambda h, c=c: h.transpose(btv[:, o0:o0 + 128], hb[:, c * 128:(c + 1) * 128], ident[:, :]),
                             reads=[hbk, "ident"], writes=[bkey])
                    T.append(P.end_capture())
                    P.capture()
                    P.op("act", lambda h: h.activation(out=xs[:, :], in_=xs[:, :], func=AF.Identity, scale=mvt[:, 2:3], bias=mvt[:, 3:4]),
                         reads=[xsk, mk_], writes=[xsk])
                    P.op("pool", lambda h: h.tensor_tensor(out=xs[:, :], in0=xs[:, :], in1=lng[:, :], op=ALU.mult),
                         reads=[xsk, "lng"], writes=[xsk])
                    P.op("pool", lambda h: h.tensor_tensor(out=h1[:, t, :], in0=xs[:, :], in1=lnb[:, :], op=ALU.add),
                         reads=[xsk, "lnb"], writes=[("h1", t)])
                    F.append(P.end_capture())
                items = A[0] + A[1] + H[0] + A[2] + H[1] + F[0] + A[3] + H[2] + F[1] + T[0] + H[3] + F[2] + T[1] + T[2] + T[3] + F[3]
                P.capture()
                for c in range(8):
                    bank, bkey = bq[c // 2]
                    src = bank[:, :].bitcast(BF16)[:, (c % 2) * 512:(c % 2) * 512 + 512]
                    if c % 2 == 0:
                        P.op("act", lambda h: h.activation(out=h1T[:, c, :], in_=src, func=AF.Identity, scale=lncol[:, c:c + 1], bias=lncol[:, 8 + c:9 + c]),
                             reads=[bkey, "lncol"], writes=a3k([c]))
                    else:
                        P.op("dve", lambda h: h.tensor_scalar(out=h1T[:, c, :], in0=src, scalar1=lncol[:, c:c + 1], scalar2=lncol[:, 8 + c:9 + c],
                                                              op0=ALU.mult, op1=ALU.add), reads=[bkey, "lncol"], writes=a3k([c]))
                items += P.end_capture()
                return items
            region(build_ln1)

            for i in range(6):
                nf = 4 if i < 5 else 2
                wg_, wgk = wget()
                wu_, wuk = wget(live=2)
                if i == 1:
                    load_ln(2)
                for j in range(nf):
                    f = 4 * i + j
                    bg, bgk = nb()
                    mm_acc(bg[:, :], bgk, lambda kc, j=j: wg_[:, kc, j * 128:(j + 1) * 128], lambda kc: h1T[:, kc, :], 8, [wgk] + a3k(ALL8))
                    bu, buk = nb()
                    mm_acc(bu[:, :], buk, lambda kc, j=j: wu_[:, kc, j * 128:(j + 1) * 128], lambda kc: h1T[:, kc, :], 8, [wuk] + a3k(ALL8))
                    sg, sgk = n32()
                    P.op("act", lambda h, bg=bg, sg=sg: h.activation(out=sg[:, :], in_=bg[:, :], func=AF.Silu), reads=[bgk], writes=[sgk])
                    P.op("dve", lambda h, bu=bu, sg=sg, f=f: h.tensor_tensor(out=aT[:, f, :], in0=bu[:, :], in1=sg[:, :], op=ALU.mult),
                         reads=[buk, sgk], writes=a5k([f]))

            b8 = [nb() for _ in range(8)]
            for i in range(6):
                nf = 4 if i < 5 else 2
                wd_, wdk = wget()
                for t in range(4):
                    tsl = slice(t * 128, (t + 1) * 128)
                    for half in range(2):
                        bank, bkey = b8[2 * t + half]
                        for j in range(nf):
                            f = 4 * i + j
                            P.op("pe", lambda h, bank=bank, f=f, j=j, tsl=tsl, half=half, wd_=wd_: h.matmul(bank[:, :], lhsT=aT[:, f, tsl], rhs=wd_[:, j, half * 512:(half + 1) * 512],
                                                                                                       start=(f == 0), stop=(f == NF - 1)),
                                 reads=[wdk] + a5k([f]), writes=[bkey])

            yss = []
            for t in range(4):
                ys, ysk, ysi = n4k()
                yss.append((ys, ysk, ysi))
                for half in range(2):
                    bank, bkey = b8[2 * t + half]
                    P.op("dve", lambda h, bank=bank, ys=ys, half=half, t=t: h.scalar_tensor_tensor(out=ys[:, half * 512:(half + 1) * 512], in0=h1[:, t, half * 512:(half + 1) * 512],
                                                                                                  scalar=ALPHA, in1=bank[:, :], op0=ALU.mult, op1=ALU.add),
                         reads=[bkey, ("h1", t)], writes=[ysk])
            chains = []
            for t in range(4):
                ys, ysk, ysi = yss[t]
                P.capture()
                mvt, mk_ = ln_stats(ys, ysk, t)
                ln_affine(ys, ysk, ys[:, :], ysk, mvt, mk_)
                okey = ("out", b, t)
                P.dma("sp", lambda h: h.dma_start(out=out_d[b * TB + t * 128:b * TB + (t + 1) * 128, :], in_=ys[:, :]),
                      reads=[ysk], writes=[okey], semkey=("t4k_out", ysi))
                out_keys.append(okey)
                chains.append(P.end_capture())
            pending["ln2"] = merge_skew(chains, 4)

        P.replay(pending["ln2"])
        P.final_wait("sp", out_keys)
        stats = P.emit()
    return nc, stats


def _kmajor(w, nk):
    return np.ascontiguousarray(w.reshape(nk, 128, w.shape[1]).transpose(1, 0, 2))


def _consts():
    c = np.zeros((128, NCST), np.float32)
    c[:, 0:128] = np.eye(128, dtype=np.float32)
    perm = np.zeros((128, 128), np.float32)
    for base in (0, 64):
        for i in range(8):
            perm[base + i + 8, base + i] = -1.0
            perm[base + i, base + i + 8] = 1.0
    c[:, 128:256] = perm
    k = np.arange(128)[:, None]
    q = np.arange(128)[None, :]
    le = (k <= q).astype(np.float32)
    gt = (k > q).astype(np.float32)
    c[:, 256:384] = le
    c[:, 384:512] = gt
    c[:, 512:640] = le * np.float32(-1.0 / 16)
    c[:, 640:768] = gt * np.float32(-1.0 / 16)
    inv_freq = (np.float32(ROPE_THETA) ** (-(np.arange(0, 16, 2, dtype=np.float32)) / np.float32(16))).astype(np.float32)
    invf = np.zeros(128, np.float32)
    for base in (0, 64):
        for i in range(16):
            invf[base + i] = inv_freq[i % 8]
    c[:, 768] = invf
    return c


def prep_shared(inp):
    f = lambda a: np.asarray(a, dtype=np.float32)
    w_in = f(inp["w_in"])[0]
    cols = {}
    pts = np.cumsum([0, 512, 512, 1024, 1024, 16, 1024, 128, 128, 1024, 1024])
    names = ["qa", "ka", "va", "ra", "dl", "qb", "kb", "vb", "ga", "gb"]
    for i, nme in enumerate(names):
        cols[nme] = w_in[:, pts[i]:pts[i + 1]]
    qb = cols["qb"].reshape(D, 16, 64)
    qb_perm = np.concatenate([np.concatenate([qb[:, p], qb[:, 8 + p]], axis=1) for p in range(8)], axis=1)
    win = np.concatenate([cols["qa"], cols["ka"], cols["va"], cols["ra"], qb_perm, cols["ga"], cols["gb"],
                          cols["kb"], cols["vb"], cols["dl"]], axis=1)
    assert win.shape == (D, 6416)
    wb = f(inp["w_branch_b"])[0].reshape(16, 64, D)
    wb_perm = np.concatenate([np.concatenate([wb[p], wb[8 + p]], axis=0) for p in range(8)], axis=0)
    sinks = f(inp["sinks"])[0]
    sk = np.zeros((128, 8), np.float32)
    sk[0:64, :] = sinks[None, 0:8]
    sk[64:128, :] = sinks[None, 8:16]
    shared = {
        "win": _kmajor(win, 8),
        "wa": _kmajor(f(inp["w_branch_a"])[0], 8),
        "wb": _kmajor(wb_perm, 8),
        "wo": _kmajor(f(inp["w_out"])[0], 8),
        "wg": _kmajor(f(inp["w_ffn_gate"])[0], 8),
        "wu": _kmajor(f(inp["w_ffn_up"])[0], 8),
        "wd": _kmajor(f(inp["w_ffn_down"])[0], NF),
        "wdec": np.ascontiguousarray(np.concatenate([f(inp["w_decay_up"])[0], f(inp["b_decay"])[0][None, :]], axis=0)),
        "gbc": np.ascontiguousarray(np.tile(f(inp["gla_norm_g"])[0], 4)[None, :]),
        "lnp": np.ascontiguousarray(np.stack([f(inp["ln1_g"])[0], f(inp["ln1_b"])[0], f(inp["ln2_g"])[0], f(inp["ln2_b"])[0]], axis=0)),
        "sinks8": sk,
        "cst": _consts(),
        "lncol": np.ascontiguousarray(np.concatenate([f(inp["ln1_g"])[0].reshape(8, 128).T, f(inp["ln1_b"])[0].reshape(8, 128).T], axis=1)),
    }
    return shared


_CACHE = {}


def kernel(**inputs):
    x = np.asarray(inputs["x"], dtype=np.float32)
    pos = np.asarray(inputs["positions"], dtype=np.int32)
    B, T, _ = x.shape
    NB = T // TB
    if NB not in _CACHE:
        _CACHE[NB] = build_program(NB)[0]
    nc = _CACHE[NB]
    shared = prep_shared(inputs)
    in_maps = []
    for i in range(B):
        m = dict(shared)
        m["x"] = np.ascontiguousarray(x[i])
        m["pos"] = np.ascontiguousarray(pos[i][None, :])
        in_maps.append(m)
    res = run_bass_kernel_spmd(nc, in_maps, core_ids=list(range(B)))
    out = np.stack([np.asarray(r["out"]) for r in res.results], axis=0).astype(np.float32)
    return out
```
